# Optimizing a Trainium2 kernel written in Bass

```python
import math
import jax, jax.numpy as jnp
from jax import lax
import numpy as np

D_MODEL = 1024
BATCH = 16
SEQ = 2048
DEPTH = 2

GRID_W = 64
HEAD_DIM = 64
A_HEADS = 8
A_KV_HEADS = 2
B_HEADS = 8
B_KV_HEADS = 2
WINDOW = 128
C_HEADS = 8
C_NOPE_DIM = 64
C_ROPE_DIM = 32
C_V_DIM = 64
C_Q_LORA = 384
C_KV_LORA = 256
Q_BLOCK = 128
N_BUCKETS = 32
MAX_DISTANCE = 128
N_EXPERTS = 16
N_GROUPS = 4
EXPERTS_PER_GROUP = N_EXPERTS // N_GROUPS
TOP_K = 2
D_FF_EXPERT = 512
ROPE_THETA = 10000.0
NORM_EPS = 1e-6
NEG_INF = -1e30
DEEPNORM_ALPHA = (2 * DEPTH) ** 0.25
DEEPNORM_BETA = (8 * DEPTH) ** -0.25
A_Q_W = A_HEADS * HEAD_DIM
A_KV_W = A_KV_HEADS * HEAD_DIM
B_Q_W = B_HEADS * HEAD_DIM
B_KV_W = B_KV_HEADS * HEAD_DIM
IN_SPLITS = (A_Q_W, A_KV_W, A_KV_W, B_Q_W, B_KV_W, B_KV_W,
             C_Q_LORA, C_KV_LORA, C_ROPE_DIM, D_MODEL, D_MODEL, D_MODEL)
IN_COLS = sum(IN_SPLITS)
A_OUT_W = A_HEADS * HEAD_DIM
B_OUT_W = B_HEADS * HEAD_DIM
C_OUT_W = C_HEADS * C_V_DIM

kernel_name = 'hybrid_gated_grid_window_mla_grouped_moe_encoder'


def layer_norm(x, g, b):
    xf = x.astype(jnp.float32)
    mu = jnp.mean(xf, axis=-1, keepdims=True)
    var = jnp.mean(jnp.square(xf - mu), axis=-1, keepdims=True)
    y = (xf - mu) * lax.rsqrt(var + NORM_EPS) * g.astype(jnp.float32) + b.astype(jnp.float32)
    return y.astype(x.dtype)


def rms_norm(x, g):
    xf = x.astype(jnp.float32)
    y = xf * lax.rsqrt(jnp.mean(jnp.square(xf), axis=-1, keepdims=True) + NORM_EPS)
    return (y * g.astype(jnp.float32)).astype(x.dtype)


def rope_cos_sin(pos, dim):
    inv = ROPE_THETA ** (-(jnp.arange(0, dim, 2, dtype=jnp.float32) / dim))
    ang = pos.astype(jnp.float32)[:, None] * inv[None, :]
    return jnp.cos(ang), jnp.sin(ang)


def apply_rope(x, cos, sin):
    half = x.shape[-1] // 2
    x1, x2 = x[..., :half], x[..., half:]
    c = cos[:, None, :].astype(x.dtype)
    s = sin[:, None, :].astype(x.dtype)
    return jnp.concatenate([x1 * c - x2 * s, x2 * c + x1 * s], axis=-1)


def axial_rope(x, row_cs, col_cs):
    half = x.shape[-1] // 2
    return jnp.concatenate([apply_rope(x[..., :half], *row_cs),
                            apply_rope(x[..., half:], *col_cs)], axis=-1)


def t5_bucket(rel):
    nb = N_BUCKETS // 2
    ret = (rel > 0).astype(np.int32) * nb
    n = np.abs(rel)
    max_exact = nb // 2
    large = max_exact + (np.log(np.maximum(n, 1) / max_exact)
                         / math.log(MAX_DISTANCE / max_exact) * (nb - max_exact)).astype(np.int32)
    large = np.minimum(large, nb - 1)
    return (ret + np.where(n < max_exact, n, large)).astype(np.int32)


def band_relative_bias(rpb_table):
    span = Q_BLOCK + 2 * WINDOW
    rel = np.arange(span)[None, :] - WINDOW - np.arange(Q_BLOCK)[:, None]
    bias = rpb_table.astype(jnp.float32)[t5_bucket(rel)]
    bias = jnp.transpose(bias, (2, 0, 1))
    return bias.reshape(B_KV_HEADS, B_HEADS // B_KV_HEADS, Q_BLOCK, span)


def dense_block_attention(q, k, v, scale):
    b, s, kvh, g, dq = q.shape
    nb = s // Q_BLOCK
    qb = q.reshape(b, nb, Q_BLOCK, kvh, g, dq).transpose(1, 0, 2, 3, 4, 5)

    def one(qblk):
        logits = jnp.einsum('bqkgd,bskd->bkgqs', qblk, k,
                            preferred_element_type=jnp.float32) * scale
        p = jax.nn.softmax(logits, axis=-1).astype(v.dtype)
        return jnp.einsum('bkgqs,bskd->bqkgd', p, v)

    out = lax.map(one, qb)
    return out.transpose(1, 0, 2, 3, 4, 5).reshape(b, s, kvh, g, v.shape[-1])


def windowed_sink_attention(q, k, v, sink, bias, scale):
    b, s, kvh, g, d = q.shape
    nb = s // Q_BLOCK
    span = Q_BLOCK + 2 * WINDOW
    pad = ((0, 0), (WINDOW, WINDOW), (0, 0), (0, 0))
    kp = jnp.pad(k, pad)
    vp = jnp.pad(v, pad)
    qb = q.reshape(b, nb, Q_BLOCK, kvh, g, d).transpose(1, 0, 2, 3, 4, 5)
    rel = np.arange(span)[None, :] - WINDOW - np.arange(Q_BLOCK)[:, None]
    band = jnp.asarray(np.abs(rel) <= WINDOW)
    sink_f = sink.astype(jnp.float32)[None, :, :, None, None]

    def one(args):
        i, qblk = args
        start = i * Q_BLOCK
        kb = lax.dynamic_slice_in_dim(kp, start, span, axis=1)
        vb = lax.dynamic_slice_in_dim(vp, start, span, axis=1)
        key_pos = start - WINDOW + jnp.arange(span)
        valid = band & ((key_pos >= 0) & (key_pos < s))[None, :]
        logits = jnp.einsum('bqkgd,bskd->bkgqs', qblk, kb,
                            preferred_element_type=jnp.float32) * scale + bias[None]
        logits = jnp.where(valid, logits, NEG_INF)
        sink_col = jnp.broadcast_to(sink_f, logits.shape[:-1] + (1,))
        p = jax.nn.softmax(jnp.concatenate([logits, sink_col], axis=-1), axis=-1)
        p = p[..., :span].astype(v.dtype)
        return jnp.einsum('bkgqs,bskd->bqkgd', p, vb)

    out = lax.map(one, (jnp.arange(nb), qb))
    return out.transpose(1, 0, 2, 3, 4, 5).reshape(b, s, kvh, g, d)


def token_mixer(h, w_in, a_qn_g, a_kn_g, b_sink, rpb_bias, c_qn_g, c_kvn_g, c_w_uq, c_w_ukv,
                w_br_a, w_br_b, w_br_c, w_o, row_cs, col_cs, seq_cs):
    b, s, _ = h.shape
    proj = h @ w_in
    parts = []
    off = 0
    for width in IN_SPLITS:
        parts.append(proj[..., off:off + width])
        off += width
    qa, ka, va, qb, kb, vb, cq, ckv, kr, ga, gb, gc = parts

    qa = axial_rope(rms_norm(qa.reshape(b, s, A_HEADS, HEAD_DIM), a_qn_g), row_cs, col_cs)
    ka = axial_rope(rms_norm(ka.reshape(b, s, A_KV_HEADS, HEAD_DIM), a_kn_g), row_cs, col_cs)
    va = va.reshape(b, s, A_KV_HEADS, HEAD_DIM)
    qa = qa.reshape(b, s, A_KV_HEADS, A_HEADS // A_KV_HEADS, HEAD_DIM)
    out_a = dense_block_attention(qa, ka, va, HEAD_DIM ** -0.5).reshape(b, s, A_OUT_W)

    qb = qb.reshape(b, s, B_KV_HEADS, B_HEADS // B_KV_HEADS, HEAD_DIM)
    kb = kb.reshape(b, s, B_KV_HEADS, HEAD_DIM)
    vb = vb.reshape(b, s, B_KV_HEADS, HEAD_DIM)
    sink = b_sink.reshape(B_KV_HEADS, B_HEADS // B_KV_HEADS)
    out_b = windowed_sink_attention(qb, kb, vb, sink, rpb_bias,
                                    HEAD_DIM ** -0.5).reshape(b, s, B_OUT_W)

    q_c = (rms_norm(cq, c_qn_g) @ c_w_uq).reshape(b, s, C_HEADS, C_NOPE_DIM + C_ROPE_DIM)
    q_nope, q_pe = q_c[..., :C_NOPE_DIM], q_c[..., C_NOPE_DIM:]
    q_pe = apply_rope(q_pe, *seq_cs)
    kv_c = (rms_norm(ckv, c_kvn_g) @ c_w_ukv).reshape(b, s, C_HEADS, C_NOPE_DIM + C_V_DIM)
    k_nope, v_c = kv_c[..., :C_NOPE_DIM], kv_c[..., C_NOPE_DIM:]
    k_pe = apply_rope(kr.reshape(b, s, 1, C_ROPE_DIM), *seq_cs)
    k_c = jnp.concatenate([k_nope, jnp.broadcast_to(k_pe, (b, s, C_HEADS, C_ROPE_DIM))], axis=-1)
    q_c = jnp.concatenate([q_nope, q_pe], axis=-1)[:, :, :, None, :]
    out_c = dense_block_attention(q_c, k_c, v_c,
                                  (C_NOPE_DIM + C_ROPE_DIM) ** -0.5).reshape(b, s, C_OUT_W)

    merged = (jax.nn.sigmoid(ga) * (out_a @ w_br_a)
              + jax.nn.sigmoid(gb) * (out_b @ w_br_b)
              + jax.nn.sigmoid(gc) * (out_c @ w_br_c))
    return merged @ w_o


def grouped_moe(h, router_w, router_bias, w_gate, w_up, w_down):
    b, s, d = h.shape
    t = h.reshape(b * s, d)
    scores = jax.nn.sigmoid(jnp.dot(t, router_w, preferred_element_type=jnp.float32))
    biased = scores + router_bias.astype(jnp.float32)
    grouped = biased.reshape(-1, N_GROUPS, EXPERTS_PER_GROUP)
    group_score = jnp.sum(lax.top_k(grouped, TOP_K)[0], axis=-1)
    sel = jnp.argmax(group_score, axis=-1)
    in_group = jnp.take_along_axis(grouped, sel[:, None, None], axis=1)[:, 0]
    _, local = lax.top_k(in_group, TOP_K)
    idx = sel[:, None] * EXPERTS_PER_GROUP + local
    wts = jnp.take_along_axis(scores, idx, axis=1)
    wts = wts / jnp.sum(wts, axis=-1, keepdims=True)
    combine = jnp.sum(jax.nn.one_hot(idx, N_EXPERTS, dtype=jnp.float32) * wts[..., None],
                      axis=1).astype(h.dtype)
    out = jnp.zeros_like(t)
    for e in range(N_EXPERTS):
        hid = jax.nn.silu(t @ w_gate[e]) * (t @ w_up[e])
        out = out + combine[:, e:e + 1] * (hid @ w_down[e])
    return out.reshape(b, s, d)


def setup_inputs(seed: int = 0) -> dict:
    key = jax.random.key(seed)
    ks = jax.random.split(key, 26)
    f32 = jnp.float32

    def nrm(k, shape, scale):
        return jax.random.normal(k, shape, f32) * scale

    L = DEPTH
    return {
        'x': nrm(ks[0], (BATCH, SEQ, D_MODEL), 1.0),
        'ln_in_g': 1.0 + nrm(ks[1], (D_MODEL,), 0.02),
        'ln_in_b': nrm(ks[2], (D_MODEL,), 0.02),
        'w_in': nrm(ks[3], (L, D_MODEL, IN_COLS), D_MODEL ** -0.5),
        'a_q_norm_g': 1.0 + nrm(ks[4], (L, HEAD_DIM), 0.02),
        'a_k_norm_g': 1.0 + nrm(ks[5], (L, HEAD_DIM), 0.02),
        'b_sink': nrm(ks[6], (L, B_HEADS), 0.5),
        'rpb_table': nrm(ks[7], (N_BUCKETS, B_HEADS), 0.5),
        'c_q_norm_g': 1.0 + nrm(ks[8], (L, C_Q_LORA), 0.02),
        'c_kv_norm_g': 1.0 + nrm(ks[9], (L, C_KV_LORA), 0.02),
        'c_w_uq': nrm(ks[10], (L, C_Q_LORA, C_HEADS * (C_NOPE_DIM + C_ROPE_DIM)), C_Q_LORA ** -0.5),
        'c_w_ukv': nrm(ks[11], (L, C_KV_LORA, C_HEADS * (C_NOPE_DIM + C_V_DIM)), C_KV_LORA ** -0.5),
        'w_branch_a': nrm(ks[12], (L, A_OUT_W, D_MODEL), A_OUT_W ** -0.5),
        'w_branch_b': nrm(ks[13], (L, B_OUT_W, D_MODEL), B_OUT_W ** -0.5),
        'w_branch_c': nrm(ks[14], (L, C_OUT_W, D_MODEL), C_OUT_W ** -0.5),
        'w_o': nrm(ks[15], (L, D_MODEL, D_MODEL), D_MODEL ** -0.5 * DEEPNORM_BETA),
        'ln1_g': 1.0 + nrm(ks[16], (L, D_MODEL), 0.02),
        'ln1_b': nrm(ks[17], (L, D_MODEL), 0.02),
        'router_w': nrm(ks[18], (D_MODEL, N_EXPERTS), D_MODEL ** -0.5),
        'router_bias': nrm(ks[19], (N_EXPERTS,), 0.01),
        'w_gate': nrm(ks[20], (L, N_EXPERTS, D_MODEL, D_FF_EXPERT), D_MODEL ** -0.5),
        'w_up': nrm(ks[21], (L, N_EXPERTS, D_MODEL, D_FF_EXPERT), D_MODEL ** -0.5),
        'w_down': nrm(ks[22], (L, N_EXPERTS, D_FF_EXPERT, D_MODEL), D_FF_EXPERT ** -0.5 * DEEPNORM_BETA),
        'ln2_g': 1.0 + nrm(ks[23], (L, D_MODEL), 0.02),
        'ln2_b': nrm(ks[24], (L, D_MODEL), 0.02),
    }


def reference(x, ln_in_g, ln_in_b, w_in, a_q_norm_g, a_k_norm_g, b_sink, rpb_table,
              c_q_norm_g, c_kv_norm_g, c_w_uq, c_w_ukv, w_branch_a, w_branch_b, w_branch_c,
              w_o, ln1_g, ln1_b, router_w, router_bias, w_gate, w_up, w_down, ln2_g, ln2_b):
    b, s, _ = x.shape
    n_rows = s // GRID_W
    rows = jnp.repeat(jnp.arange(n_rows), GRID_W)
    cols = jnp.tile(jnp.arange(GRID_W), n_rows)
    row_cs = rope_cos_sin(rows, HEAD_DIM // 2)
    col_cs = rope_cos_sin(cols, HEAD_DIM // 2)
    seq_cs = rope_cos_sin(jnp.arange(s), C_ROPE_DIM)
    rpb_bias = band_relative_bias(rpb_table)

    x = layer_norm(x, ln_in_g, ln_in_b)
    for l in range(DEPTH):
        mix = token_mixer(x, w_in[l], a_q_norm_g[l], a_k_norm_g[l], b_sink[l], rpb_bias,
                          c_q_norm_g[l], c_kv_norm_g[l], c_w_uq[l], c_w_ukv[l],
                          w_branch_a[l], w_branch_b[l], w_branch_c[l], w_o[l],
                          row_cs, col_cs, seq_cs)
        x = layer_norm(DEEPNORM_ALPHA * x + mix, ln1_g[l], ln1_b[l])
        ffn = grouped_moe(x, router_w, router_bias, w_gate[l], w_up[l], w_down[l])
        x = layer_norm(DEEPNORM_ALPHA * x + ffn, ln2_g[l], ln2_b[l])
    return x
```

```python
import math
import numpy as np
from contextlib import ExitStack
import concourse.bass as bass
import concourse.mybir as mybir
from concourse.bass_utils import run_bass_kernel_spmd

F32 = mybir.dt.float32
BF16 = mybir.dt.bfloat16
AF = mybir.ActivationFunctionType
ALU = mybir.AluOpType

D = 1024
SEQ = 2048
NSEQ = 2
NT = NSEQ * SEQ
TG = 512
NG = NT // TG
NTILE = NT // 128
L = 2
INC = 5280
NE = 16
FF = 512
QA0, KA0, VA0, QB0, KB0, VB0, CQ0, CKV0, KR0, GA0 = 0, 512, 640, 768, 1280, 1408, 1536, 1920, 2176, 2208
NQKV = 2208
EPS = 1e-6
ALPHA = (2 * L) ** 0.25
NEG = -30000.0


class Buf:
    __slots__ = ("name", "lw", "rd", "rd_dma")

    def __init__(self, name=""):
        self.name = name
        self.lw = None
        self.rd = {}
        self.rd_dma = []


class Op:
    __slots__ = ("eng", "fn", "deps", "signal", "sem", "semval", "isdma")

    def __init__(self, eng, fn, isdma):
        self.eng = eng
        self.fn = fn
        self.isdma = isdma
        self.deps = []
        self.signal = False
        self.sem = None
        self.semval = None


class Sched:
    ENGS = ("pe", "act", "dve", "pool", "sp")
    NDMASEM = 8

    def __init__(self, nc):
        self.nc = nc
        self.ops = {e: [] for e in self.ENGS}
        self.bar = {e: [] for e in self.ENGS}
        self.dmas_since_bar = []

    def add(self, eng, fn, reads=(), writes=(), dma=False):
        op = Op(eng, fn, dma)
        deps = {}
        for b in reads:
            if b.lw is not None:
                deps[id(b.lw)] = b.lw
        for b in writes:
            if b.lw is not None:
                deps[id(b.lw)] = b.lw
            for r in b.rd.values():
                deps[id(r)] = r
            for r in b.rd_dma:
                deps[id(r)] = r
        for d in self.bar[eng]:
            deps[id(d)] = d
        self.bar[eng] = []
        for d in deps.values():
            if (not d.isdma) and (not dma) and d.eng == eng and eng == "pe":
                continue
            d.signal = True
            op.deps.append(d)
        for b in writes:
            b.lw = op
            b.rd = {}
            b.rd_dma = []
        for b in reads:
            if dma:
                b.rd_dma.append(op)
            else:
                b.rd[eng] = op
        if dma:
            op.signal = True
            self.dmas_since_bar.append(op)
        self.ops[eng].append(op)
        return op

    def dma(self, q, out, in_, reads=(), writes=()):
        return self.add(q, lambda e: e.dma_start(out=out, in_=in_), reads, writes, dma=True)

    def barrier(self):
        lasts = []
        for e in self.ENGS:
            for o in reversed(self.ops[e]):
                if not o.isdma:
                    lasts.append(o)
                    break
        lasts += self.dmas_since_bar
        self.dmas_since_bar = []
        for e in self.ENGS:
            self.bar[e] = self.bar[e] + lasts

    def emit(self, es):
        nc = self.nc
        esem = {e: es.enter_context(nc.semaphore("tick_" + e)) for e in self.ENGS}
        dsem = {}
        for e in self.ENGS:
            if any(o.isdma for o in self.ops[e]):
                dsem[e] = [es.enter_context(nc.semaphore("dma_%s_%d" % (e, i))) for i in range(self.NDMASEM)]
        for e in self.ENGS:
            t = 0
            n = 0
            for o in self.ops[e]:
                if o.isdma:
                    o.sem = dsem[e][n % self.NDMASEM]
                    o.semval = 16 * (n // self.NDMASEM + 1)
                    n += 1
                elif o.signal:
                    t += 1
                    o.sem = esem[e]
                    o.semval = t
        block = es.enter_context(nc.Block())
        engobj = {"pe": block.tensor, "act": block.scalar, "dve": block.vector,
                  "pool": block.gpsimd, "sp": block.sync}

        def make(e):
            ops = self.ops[e]

            def body(eng):
                waited = {}
                for o in ops:
                    need = {}
                    for d in o.deps:
                        k = id(d.sem)
                        if k not in need or need[k][1] < d.semval:
                            need[k] = (d.sem, d.semval)
                    if o.isdma and o.semval > 16:
                        k = id(o.sem)
                        v = o.semval - 16
                        if k not in need or need[k][1] < v:
                            need[k] = (o.sem, v)
                    for k, (sem, v) in need.items():
                        if waited.get(k, 0) >= v:
                            continue
                        eng.wait_ge(sem, v)
                        waited[k] = v
                    ins = o.fn(eng)
                    if o.isdma:
                        ins.then_inc(o.sem, 16)
                    elif o.signal:
                        ins.then_inc(o.sem, 1)
                if e == "sp":
                    for q in self.ENGS:
                        last = {}
                        for o in self.ops[q]:
                            if o.isdma:
                                last[id(o.sem)] = (o.sem, o.semval)
                        for sem, v in last.values():
                            eng.wait_ge(sem, v)
            return body

        for e in self.ENGS:
            if self.ops[e] or e == "sp":
                engobj[e](make(e))


class Arena:
    def __init__(self, nc, es, nbytes):
        self.t32 = es.enter_context(nc.sbuf_tensor("arena", [128, nbytes // 4], F32))
        self.t16 = self.t32.bitcast(BF16)
        self.top = 0
        self.cap = nbytes
        self.peak = 0

    def alloc(self, dtype, shape):
        n = 1
        for s in shape:
            n *= s
        esz = 4 if dtype == F32 else 2
        nb = (n * esz + 63) // 64 * 64
        off = self.top
        self.top += nb
        self.peak = max(self.peak, self.top)
        assert self.top <= self.cap, "SBUF arena overflow %d > %d" % (self.top, self.cap)
        base = self.t32 if dtype == F32 else self.t16
        ap = base[:, off // esz: off // esz + n]
        if len(shape) == 2:
            ap = ap.rearrange("p (a b) -> p a b", a=shape[0])
        elif len(shape) == 3:
            ap = ap.rearrange("p (a b c) -> p a b c", a=shape[0], b=shape[1])
        return ap

    def at(self, off, dtype, shape):
        n = 1
        for x in shape:
            n *= x
        esz = 4 if dtype == F32 else 2
        assert off % 64 == 0 and off + n * esz <= self.cap
        base = self.t32 if dtype == F32 else self.t16
        ap = base[:, off // esz: off // esz + n]
        if len(shape) == 2:
            ap = ap.rearrange("p (a b) -> p a b", a=shape[0])
        elif len(shape) == 3:
            ap = ap.rearrange("p (a b c) -> p a b c", a=shape[0], b=shape[1])
        return ap

    def mark(self):
        return self.top

    def release(self, m):
        self.top = m


class Ring:
    def __init__(self, items):
        self.items = items
        self.i = 0

    def next(self):
        it = self.items[self.i % len(self.items)]
        self.i += 1
        return it


def bcast_rows(ap1d_tensor, offset, n, parts=128):
    return bass.AP(ap1d_tensor, offset, [[0, parts], [1, n]])


def t5_bucket(rel):
    nb = 16
    ret = (rel > 0).astype(np.int32) * nb
    n = np.abs(rel)
    max_exact = nb // 2
    large = max_exact + (np.log(np.maximum(n, 1) / max_exact) / math.log(128 / max_exact) * (nb - max_exact)).astype(np.int32)
    large = np.minimum(large, nb - 1)
    return (ret + np.where(n < max_exact, n, large)).astype(np.int32)


def host_constants():
    t = np.arange(SEQ)
    inv = (np.float32(10000.0) ** (-(np.arange(0, 32, 2, dtype=np.float32) / np.float32(32)))).astype(np.float32)
    cosA = np.zeros((128, SEQ), np.float32)
    sinA = np.zeros((128, SEQ), np.float32)
    PA = np.zeros((128, 128), np.float32)
    for p in range(128):
        d = p % 64
        half = d // 32
        dd = d % 32
        i = dd % 16
        pos = (t // 64) if half == 0 else (t % 64)
        ang = pos.astype(np.float32) * inv[i]
        cosA[p] = np.cos(ang)
        sgn = -1.0 if dd < 16 else 1.0
        sinA[p] = sgn * np.sin(ang)
        partner = p + 16 if dd < 16 else p - 16
        PA[partner, p] = 1.0
    cosC = np.ones((128, SEQ), np.float32)
    sinC = np.zeros((128, SEQ), np.float32)
    PC = np.zeros((128, 128), np.float32)
    for p in range(64, 96):
        dd = p - 64
        i = dd % 16
        ang = t.astype(np.float32) * inv[i]
        cosC[p] = np.cos(ang)
        sgn = -1.0 if dd < 16 else 1.0
        sinC[p] = sgn * np.sin(ang)
        partner = p + 16 if dd < 16 else p - 16
        PC[partner, p] = 1.0
    tabs = np.concatenate([cosA, sinA, cosC, sinC], axis=1)
    ident = np.eye(128, dtype=np.float32)
    anti = np.eye(128, dtype=np.float32)[::-1].copy()
    onesBD = np.zeros((128, 128), np.float32)
    onesBD[:64, :64] = 1.0
    onesBD[64:, 64:] = 1.0
    ones = np.ones((128, 128), np.float32)
    vsink = np.zeros((128, 128), np.float32)
    vsink[:, 64:] = 1.0
    mats = np.concatenate([ident, anti, PA, PC, onesBD, ones, vsink], axis=1)
    u = np.arange(512)
    rel = u - 256
    bk = t5_bucket(rel)
    oh = np.zeros((32, 512), np.float32)
    inband = np.abs(rel) <= 128
    oh[bk[inband], u[inband]] = 1.0
    negm = np.where(inband, 0.0, NEG).astype(np.float32)[None, :].repeat(8, axis=0)
    return {"c_tabs": tabs, "c_mats": mats, "c_oh": oh, "c_negm": np.ascontiguousarray(negm)}


def build_program(stop_after=None, debug=False):
    nc = bass.Bass("TRN2", target_bir_lowering=False)
    es = ExitStack()
    with es:
        def din(name, shape):
            return nc.dram_tensor(name, shape, F32, kind="ExternalInput").ap()

        x_in = din("x", [NT, D])
        ln_in_g = din("ln_in_g", [D])
        ln_in_b = din("ln_in_b", [D])
        w_in = din("w_in", [L, D, INC])
        a_qg = din("a_q_norm_g", [L, 64])
        a_kg = din("a_k_norm_g", [L, 64])
        b_sink = din("b_sink", [L, 8])
        rpb = din("rpb_table", [32, 8])
        c_qg = din("c_q_norm_g", [L, 384])
        c_kvg = din("c_kv_norm_g", [L, 256])
        c_wuq = din("c_w_uq", [L, 384, 768])
        c_wukv = din("c_w_ukv", [L, 256, 1024])
        w_br = [din("w_branch_a", [L, 512, D]), din("w_branch_b", [L, 512, D]), din("w_branch_c", [L, 512, D])]
        w_o = din("w_o", [L, D, D])
        ln1_g = din("ln1_g", [L, D])
        ln1_b = din("ln1_b", [L, D])
        router_w = din("router_w", [D, NE])
        router_b = din("router_bias", [NE])
        w_gate = din("w_gate", [L, NE, D, FF])
        w_up = din("w_up", [L, NE, D, FF])
        w_down = din("w_down", [L, NE, FF, D])
        ln2_g = din("ln2_g", [L, D])
        ln2_b = din("ln2_b", [L, D])
        c_tabs = din("c_tabs", [128, 4 * SEQ])
        c_mats = din("c_mats", [128, 7 * 128])
        c_oh = din("c_oh", [32, 512])
        c_negm = din("c_negm", [8, 512])
        out = nc.dram_tensor("out", [NT, D], F32, kind="ExternalOutput").ap()

        skind = "ExternalOutput" if debug else "Internal"

        def scr(name, shape, dt):
            return nc.dram_tensor(name, shape, dt, kind=skind).ap()

        xres = scr("xres", [NT, D], F32)
        qaT = scr("qaT", [4, 128, NT], BF16)
        kaT = scr("kaT", [2, 64, NT], BF16)
        vA = scr("vA", [2, NT, 64], BF16)
        qbT = scr("qbT", [4, 128, NT], BF16)
        kbT = scr("kbT", [2, 64, NT], BF16)
        vB = scr("vB", [2, NT, 64], BF16)
        qcT = scr("qcT", [8, 96, NT], BF16)
        kcT = scr("kcT", [8, 96, NT], BF16)
        vC = scr("vC", [8, NT, 64], BF16)
        oT = scr("oT", [3, 4, 128, NT], BF16)
        wtab = scr("wtab", [8, 512], F32)
        B_xres = [Buf("xres%d" % i) for i in range(NTILE)]
        B_scr = {}

        def sb(name):
            if name not in B_scr:
                B_scr[name] = Buf(name)
            return B_scr[name]

        S = Sched(nc)
        A = Arena(nc, es, 207 * 1024)
        psum = []
        P16 = {}
        for i in range(8):
            ph = es.enter_context(nc.psum_tensor("ps%d" % i, [128, 512], F32))
            pb = Buf("ps%d" % i)
            psum.append((ph[:, :], pb))
            P16[id(pb)] = ph.bitcast(BF16)[:, :]
        PS = Ring(psum)

        xTd = scr("xTd", [8, 128, NT], BF16)
        mTd = scr("mTd", [8, 128, NT], BF16)
        mats16 = A.alloc(BF16, [7, 128])
        B_mats16 = Buf("mats16")
        mats32 = A.alloc(F32, [2, 128])
        B_mats32 = Buf("mats32")
        cvec = A.alloc(F32, [8])
        B_cvec = Buf("cvec")
        S.dma("pool", mats16, c_mats.rearrange("p (a b) -> p a b", a=7), writes=[B_mats16])
        S.dma("sp", mats32, c_mats[:, 0:256].rearrange("p (a b) -> p a b", a=2), writes=[B_mats32])
        ident16, PA16, PC16, onesBD16, ones16, vsink16 = (mats16[:, 0, :], mats16[:, 2, :], mats16[:, 3, :],
                                                          mats16[:, 4, :], mats16[:, 5, :], mats16[:, 6, :])
        ident32, anti32 = mats32[:, 0, :], mats32[:, 1, :]
        S.add("pool", lambda e: e.memset(cvec[:, 0:1], EPS), writes=[B_cvec])
        S.add("pool", lambda e: e.memset(cvec[:, 1:2], -0.5), writes=[B_cvec])
        S.add("pool", lambda e: e.memset(cvec[:, 2:3], 1.0), writes=[B_cvec])
        eps_ap = cvec[:, 0:1]
        nhalf_ap = cvec[:, 1:2]
        rw = A.alloc(BF16, [8, NE])
        rbb = A.alloc(F32, [NE])
        B_rw = Buf("rw")
        S.dma("pool", rw, router_w.rearrange("(kc p) e -> p kc e", p=128), writes=[B_rw])
        S.dma("sp", rbb, bcast_rows(router_b.tensor, 0, NE), writes=[B_rw])
        comb_all = A.alloc(F32, [NTILE, NE])
        B_comb = Buf("comb")
        rt = None
        B_rt = Buf("rt")
        BIG = 1.0e4

        def mm(o, lhsT, rhs, start, stop, r, w):
            S.add("pe", lambda e: e.matmul(o, lhsT=lhsT, rhs=rhs, start=start, stop=stop), r, w)

        def act(o, i, func, r, w, scale=None, bias=None):
            kw = {}
            if scale is not None:
                kw["scale"] = scale
            if bias is not None:
                kw["bias"] = bias
            S.add("act", lambda e: e.activation(out=o, in_=i, func=func, **kw), r, w)

        def tt(eng, o, a, b, op, r, w):
            S.add(eng, lambda e: e.tensor_tensor(out=o, in0=a, in1=b, op=op), r, w)

        def ts(eng, o, a, s1, s2, op0, op1, r, w):
            S.add(eng, lambda e: e.tensor_scalar(out=o, in0=a, scalar1=s1, scalar2=s2, op0=op0, op1=op1), r, w)

        def stt(o, a, sc, b, op0, op1, r, w):
            S.add("dve", lambda e: e.scalar_tensor_tensor(out=o, in0=a, scalar=sc, in1=b, op0=op0, op1=op1), r, w)

        def layer_norm_tile(y, B_y, gbc, bbc, B_gb, tile_idx, lnw, dst_dram, xst=None, B_xst=None, t4=0, eps=EPS):
            st, B_st, xn, B_xn, xo, B_xo = lnw.next()
            for h in range(2):
                S.add("dve", lambda e, h=h: e.bn_stats(out=st[:, h * 6:(h + 1) * 6], in_=y[:, h * 512:(h + 1) * 512]),
                      [B_y], [B_st])
            S.add("dve", lambda e: e.bn_aggr(out=st[:, 12:14], in_=st[:, 0:12]), [B_st], [B_st])
            ts("dve", st[:, 14:15], st[:, 13:14], eps, None, ALU.add, ALU.bypass, [B_st], [B_st])
            tt("pool", st[:, 15:16], st[:, 14:15], nhalf_ap, ALU.pow, [B_st, B_cvec], [B_st])
            ts("dve", st[:, 16:17], st[:, 12:13], st[:, 15:16], -1.0, ALU.mult, ALU.mult, [B_st], [B_st])
            act(xn, y, AF.Identity, [B_y, B_st], [B_xn], scale=st[:, 15:16], bias=st[:, 16:17])
            tt("dve", xn, xn, gbc, ALU.mult, [B_xn, B_gb], [B_xn])
            tt("dve", xo, xn, bbc, ALU.add, [B_xn, B_gb], [B_xo])
            S.dma("sp", dst_dram[tile_idx * 128:(tile_idx + 1) * 128, :], xo, reads=[B_xo], writes=[B_xres[tile_idx]])
            if xst is not None:
                xo16, B_x16 = lnw_x16[id(B_xo)]
                act(xo16, xo, AF.Copy, [B_xo], [B_x16])
                p0, B_p0 = PS.next()
                p16 = P16[id(B_p0)]
                for kc in range(8):
                    S.add("pe", lambda e, kc=kc: e.transpose(out=p16[:, kc * 128:(kc + 1) * 128],
                                                             in_=xo16[:, kc * 128:(kc + 1) * 128], identity=ident16),
                          [B_x16, B_mats16], [B_p0])
                act(xst[:, :, t4 * 128:(t4 + 1) * 128], p16.rearrange("p (a b) -> p a b", a=8), AF.Copy, [B_p0], [B_xst])

        lnw_x16 = {}

        def make_ln_work(n=2):
            items = []
            for i in range(n):
                bxo = Buf("lnxo")
                lnw_x16[id(bxo)] = (A.alloc(BF16, [D]), Buf("lnx16"))
                items.append((A.alloc(F32, [32]), Buf("lnst"), A.alloc(F32, [D]), Buf("lnxn"), A.alloc(F32, [D]), bxo))
            return Ring(items)

        def load_gb(g_ap_tensor, g_off, b_ap_tensor, b_off):
            gbc = A.alloc(F32, [D])
            bbc = A.alloc(F32, [D])
            Bg = Buf("gb")
            S.dma("sp", gbc, bcast_rows(g_ap_tensor, g_off, D), writes=[Bg])
            S.dma("sp", bbc, bcast_rows(b_ap_tensor, b_off, D), writes=[Bg])
            return gbc, bbc, Bg

        def bc(ap2d, n, m, inner_bcast=True):
            p = list(ap2d.ap[0])
            if inner_bcast:
                return bass.AP(ap2d.tensor, ap2d.offset, [p, [1, n], [0, m]])
            return bass.AP(ap2d.tensor, ap2d.offset, [p, [0, m], [1, n]])

        def router_group(xst, B_xst, g, rt, B_rt):
            plg, B_plg = PS.next()
            for t4 in range(4):
                for kc in range(8):
                    mm(plg[:, t4 * NE:(t4 + 1) * NE], xst[:, kc, t4 * 128:(t4 + 1) * 128], rw[:, kc, :], kc == 0, kc == 7,
                       [B_xst, B_rw], [B_plg])
            R = [B_rt]
            th = rt[:, 0, :]
            sc = rt[:, 1, :]
            b = rt[:, 2, :]
            act(th, plg[:, 0:64], AF.Tanh, [B_plg], R, scale=0.5)
            ts("dve", sc, th, 0.5, 0.5, ALU.mult, ALU.add, R, R)
            tt("dve", b.rearrange("p (t e) -> p t e", t=4), sc.rearrange("p (t e) -> p t e", t=4), bc(rbb, NE, 4, inner_bcast=False),
               ALU.add, R + [B_rw], R)
            b3 = b.rearrange("p (g k) -> p g k", k=4)
            m1 = rt[:, 3, 0:16]
            m2 = rt[:, 3, 16:32]
            gs = rt[:, 3, 32:48]
            gmax = rt[:, 3, 48:52]
            S.add("dve", lambda e: e.tensor_reduce(out=m1, in_=b3, axis=mybir.AxisListType.X, op=ALU.max), R, R)
            eq = rt[:, 4, :].rearrange("p (g k) -> p g k", k=4)
            tt("dve", eq, b3, bc(m1, 16, 4), ALU.is_equal, R, R)
            b2 = rt[:, 5, :].rearrange("p (g k) -> p g k", k=4)
            stt(b2, eq, -BIG, b3, ALU.mult, ALU.add, R, R)
            S.add("dve", lambda e: e.tensor_reduce(out=m2, in_=b2, axis=mybir.AxisListType.X, op=ALU.max), R, R)
            tt("dve", gs, m1, m2, ALU.add, R, R)
            gs3 = gs.rearrange("p (t g) -> p t g", g=4)
            S.add("dve", lambda e: e.tensor_reduce(out=gmax, in_=gs3, axis=mybir.AxisListType.X, op=ALU.max), R, R)
            gsel = rt[:, 6, 0:16]
            tt("dve", gsel.rearrange("p (t g) -> p t g", g=4), gs3, bc(gmax, 4, 4), ALU.is_equal, R, R)
            selm = rt[:, 7, :]
            selm3 = selm.rearrange("p (g k) -> p g k", k=4)
            tt("dve", selm3, b3, bc(m2, 16, 4), ALU.is_ge, R, R)
            tt("dve", selm3, selm3, bc(gsel, 16, 4), ALU.mult, R, R)
            wv = rt[:, 8, :]
            tt("dve", wv, selm, sc, ALU.mult, R, R)
            wsum = rt[:, 6, 16:20]
            rws = rt[:, 6, 20:24]
            wv3 = wv.rearrange("p (t e) -> p t e", t=4)
            S.add("dve", lambda e: e.tensor_reduce(out=wsum, in_=wv3, axis=mybir.AxisListType.X, op=ALU.add), R, R)
            S.add("dve", lambda e: e.reciprocal(out=rws, in_=wsum), R, R)
            stt(comb_all[:, g * 4:(g + 1) * 4, :], wv3, 0.5, bc(rws, 4, NE), ALU.mult, ALU.mult, R, [B_comb])

        def router_tile(lhs_of, Breads, tg_idx):
            plg, B_plg = PS.next()
            for kc in range(8):
                mm(plg[:, 0:NE], lhs_of(kc), rw[:, kc, :], kc == 0, kc == 7, Breads + [B_rw], [B_plg])
            R = [B_rt]
            sc = rt[:, 0, :]
            bsc = rt[:, 1, :]
            b44 = bsc.rearrange("p (g k) -> p g k", k=4)
            act(rt[:, 2, :], plg[:, 0:NE], AF.Tanh, [B_plg], R, scale=0.5)
            ts("dve", sc, rt[:, 2, :], 0.5, 0.5, ALU.mult, ALU.add, R, R)
            tt("dve", bsc, sc, rbb, ALU.add, R + [B_rw], R)
            m1 = rt[:, 3, 0:4]
            S.add("dve", lambda e, m1=m1, b44=b44: e.tensor_reduce(out=m1, in_=b44, axis=mybir.AxisListType.X, op=ALU.max), R, R)
            m1b = bass.AP(m1.tensor, m1.offset, [list(m1.ap[0]), [1, 4], [0, 4]])
            eq = rt[:, 4, :].rearrange("p (g k) -> p g k", k=4)
            tt("dve", eq, b44, m1b, ALU.is_equal, R, R)
            b2 = rt[:, 5, :].rearrange("p (g k) -> p g k", k=4)
            stt(b2, eq, -BIG, b44, ALU.mult, ALU.add, R, R)
            m2 = rt[:, 3, 4:8]
            S.add("dve", lambda e, m2=m2, b2=b2: e.tensor_reduce(out=m2, in_=b2, axis=mybir.AxisListType.X, op=ALU.max), R, R)
            gs = rt[:, 3, 8:12]
            tt("dve", gs, m1, m2, ALU.add, R, R)
            gmax = rt[:, 3, 12:13]
            S.add("dve", lambda e, gmax=gmax, gs=gs: e.tensor_reduce(out=gmax, in_=gs, axis=mybir.AxisListType.X, op=ALU.max), R, R)
            gsel = rt[:, 6, 0:4]
            ts("dve", gsel, gs, gmax, None, ALU.is_equal, ALU.bypass, R, R)
            gselb = bass.AP(gsel.tensor, gsel.offset, [list(gsel.ap[0]), [1, 4], [0, 4]])
            pen = rt[:, 7, :].rearrange("p (g k) -> p g k", k=4)
            ts("dve", pen, gselb, BIG, -BIG, ALU.mult, ALU.add, R, R)
            bm = rt[:, 8, :]
            tt("dve", bm, bsc, rt[:, 7, :], ALU.add, R, R)
            mx8 = rt[:, 9, 0:8]
            S.add("dve", lambda e, mx8=mx8, bm=bm: e.max(out=mx8, in_=bm), R, R)
            selm = rt[:, 10, :]
            ts("dve", selm, bm, rt[:, 9, 1:2], None, ALU.is_ge, ALU.bypass, R, R)
            wv = rt[:, 11, :]
            tt("dve", wv, selm, sc, ALU.mult, R, R)
            wsum = rt[:, 6, 4:5]
            S.add("dve", lambda e, wsum=wsum, wv=wv: e.tensor_reduce(out=wsum, in_=wv, axis=mybir.AxisListType.X, op=ALU.add), R, R)
            rws = rt[:, 6, 5:6]
            S.add("dve", lambda e, rws=rws, wsum=wsum: e.reciprocal(out=rws, in_=wsum), R, R)
            ts("dve", comb_all[:, tg_idx, :], wv, rws, 0.5, ALU.mult, ALU.mult, R, [B_comb])

        import os
        SKIP01 = os.environ.get('DBG_SKIP01') == '1'
        m0 = A.mark()
        gbc, bbc, B_gb = load_gb(ln_in_g.tensor, 0, ln_in_b.tensor, 0)
        lnw = make_ln_work(2)
        xin = Ring([(A.alloc(F32, [D]), Buf("xin%d" % i)) for i in range(3)])
        xstr = Ring([(A.alloc(BF16, [8, TG]), Buf("xst%d" % i)) for i in range(2)])
        for g in range(0 if SKIP01 else NG):
            xst, B_xst = xstr.next()
            for t4 in range(4):
                t = g * 4 + t4
                xt_, B_xt = xin.next()
                S.dma("sp", xt_, x_in[t * 128:(t + 1) * 128, :], writes=[B_xt])
                layer_norm_tile(xt_, B_xt, gbc, bbc, B_gb, t, lnw, xres, xst, B_xst, t4)
            S.dma("pool", xTd[:, :, g * TG:(g + 1) * TG].rearrange("k p t -> p k t"), xst, reads=[B_xst], writes=[sb("xTd%d" % (g // 4))])
        S.barrier()
        A.release(m0)

        m0 = A.mark()
        rp32 = A.alloc(F32, [8])
        oh32 = A.alloc(F32, [512])
        ng32 = A.alloc(F32, [512])
        wt32 = A.alloc(F32, [512])
        B_t5 = Buf("t5")
        S.dma("sp", rp32[0:32, :], rpb, writes=[B_t5])
        S.dma("sp", oh32[0:32, :], c_oh, writes=[B_t5])
        S.dma("sp", ng32[0:8, :], c_negm, writes=[B_t5])
        pt, B_pt = PS.next()
        mm(pt[0:8, :], rp32[0:32, :], oh32[0:32, :], True, True, [B_t5], [B_pt])
        tt("dve", wt32[0:8, :], pt[0:8, :], ng32[0:8, :], ALU.add, [B_pt, B_t5], [B_t5])
        S.dma("sp", wtab, wt32[0:8, :], reads=[B_t5], writes=[sb("wtab")])
        S.barrier()
        A.release(m0)

        def build_biasT(biasT, B_biasT):
            hkr = Ring([(A.alloc(F32, [128]), Buf("hk%d" % i)) for i in range(2)])
            for j in range(2):
                for c in range(3):
                    for hh in range(2):
                        for pl in range(2):
                            h = 4 * j + 2 * pl + hh
                            base = 128 * (c - 1) + 129
                            hk, B_hk = hkr.next()
                            S.dma("sp", hk, bass.AP(wtab.tensor, h * 512 + base, [[1, 128], [1, 128]]), reads=[sb("wtab")], writes=[B_hk])
                            pt, B_pt = PS.next()
                            mm(pt[:, 0:128], hk, anti32, True, True, [B_hk, B_mats32], [B_pt])
                            col = (c * 4 + pl * 2 + hh) * 128
                            act(biasT[:, j, col:col + 128], pt[:, 0:128], AF.Copy, [B_pt], [B_biasT])
        if stop_after == "p0":
            S.emit(es)
            return nc

        for l in range(L):
            m1 = A.mark()
            wqkv = A.alloc(BF16, [8, NQKV])
            B_wqkv = [Buf("wqkv%d" % i) for i in range(3)]
            w_l = w_in[l].rearrange("(kc p) c -> p kc c", p=128)
            cuts = [(0, 768), (768, 1536), (1536, NQKV)]

            def wq_buf(c0):
                return B_wqkv[0] if c0 < 768 else (B_wqkv[1] if c0 < 1536 else B_wqkv[2])
            for i, (c0, c1) in enumerate(cuts):
                S.dma("pool", wqkv[:, :, c0:c1], w_l[:, :, c0:c1], writes=[B_wqkv[i]])
            wkrp = A.alloc(BF16, [8, 96])
            B_wkrp = Buf("wkrp")
            S.add("pool", lambda e: e.memset(wkrp, 0.0), writes=[B_wkrp])
            S.dma("pool", wkrp[:, :, 64:96], w_l[:, :, KR0:KR0 + 32], writes=[B_wkrp])
            wuq = A.alloc(BF16, [3, 768])
            wukv = A.alloc(BF16, [2, 1024])
            B_wu = Buf("wu")
            S.dma("pool", wuq, c_wuq[l].rearrange("(kc p) c -> p kc c", p=128), writes=[B_wu])
            S.dma("pool", wukv, c_wukv[l].rearrange("(kc p) c -> p kc c", p=128), writes=[B_wu])
            tabs = A.alloc(F32, [4, SEQ])
            B_tabs = Buf("tabs")
            S.dma("sp", tabs, c_tabs.rearrange("p (a b) -> p a b", a=4), writes=[B_tabs])
            gv = A.alloc(F32, [8])
            B_gv = Buf("gv")
            for hf in range(2):
                S.dma("sp", gv[hf * 64:(hf + 1) * 64, 0:1], bass.AP(a_qg.tensor, l * 64, [[1, 64], [1, 1]]), writes=[B_gv])
                S.dma("sp", gv[hf * 64:(hf + 1) * 64, 1:2], bass.AP(a_kg.tensor, l * 64, [[1, 64], [1, 1]]), writes=[B_gv])
            for c in range(3):
                S.dma("sp", gv[:, 2 + c:3 + c], bass.AP(c_qg.tensor, l * 384 + c * 128, [[1, 128], [1, 1]]), writes=[B_gv])
            for c in range(2):
                S.dma("sp", gv[:, 5 + c:6 + c], bass.AP(c_kvg.tensor, l * 256 + c * 128, [[1, 128], [1, 1]]), writes=[B_gv])

            w512 = Ring([(A.alloc(F32, [512]), Buf("w512_%d" % i)) for i in range(8)])
            h512 = Ring([(A.alloc(BF16, [512]), Buf("h512_%d" % i)) for i in range(8)])
            cqn = A.alloc(BF16, [3, 512])
            B_cqn = Buf("cqn")
            ckvn = A.alloc(BF16, [2, 512])
            B_ckvn = Buf("ckvn")
            sq3 = A.alloc(BF16, [3, 512])
            B_sq3 = Buf("sq3")
            vst = Ring([(A.alloc(BF16, [4, 256]), Buf("vst%d" % i)) for i in range(2)])
            vcst = Ring([(A.alloc(BF16, [512]), Buf("vcst%d" % i)) for i in range(2)])
            xgr = Ring([(A.alloc(BF16, [8, TG]), Buf("xg%d" % i)) for i in range(2)])

            class FreeList:
                def __init__(self, items):
                    self.free = list(items)

            PSF = FreeList(psum)
            WF = FreeList(w512.items)
            HF = FreeList(h512.items)
            sq3k = A.alloc(BF16, [2, 512])
            B_sq3k = Buf("sq3k")

            def acq(fl, n=1):
                while len(fl.free) < n:
                    yield
                return [fl.free.pop(0) for _ in range(n)]

            def rel(fl, *items):
                for it in items:
                    fl.free.append(it)

            def run_gens(queue, width=4):
                active = []
                idle_rounds = 0
                while queue or active:
                    while len(active) < width and queue:
                        active.append(queue.pop(0))
                    n0 = sum(len(S.ops[e]) for e in S.ENGS)
                    for gen in list(active):
                        try:
                            next(gen)
                        except StopIteration:
                            active.remove(gen)
                    n1 = sum(len(S.ops[e]) for e in S.ENGS)
                    idle_rounds = idle_rounds + 1 if n1 == n0 else 0
                    assert idle_rounds < 1000, "generator scheduler deadlock"

            for g in range(0 if SKIP01 else NG):
                t0 = g * TG
                pos0 = (g % (SEQ // TG)) * TG
                xT, B_xg = xgr.next()
                S.dma("sp", xT, xTd[:, :, t0:t0 + TG].rearrange("k p t -> p k t"), reads=[sb("xTd%d" % (g // 4))], writes=[B_xg])
                queue = []

                def projmm(p, Bp, ncols, lhs_of, rhs_of, kcs, r):
                    for kc in range(kcs):
                        mm(p[0:ncols, :], lhs_of(kc), rhs_of(kc), kc == 0, kc == kcs - 1, r, [Bp])

                def xrhs(kc, xT=xT):
                    return xT[:, kc, :]

                def rope_gen(qn, B_qn, rows, perm, cosi, sini, pos0=pos0):
                    lo, hi = rows
                    (pr, B_pr), = yield from acq(PSF, 1)
                    mm(pr[0:hi, :], perm[0:hi, 0:hi], qn[0:hi, :], True, True, [B_qn, B_mats16], [B_pr])
                    (t1, B_t1), (t2, B_t2) = yield from acq(WF, 2)
                    tt("dve", t1[lo:hi, :], qn[lo:hi, :], tabs[lo:hi, cosi, pos0:pos0 + TG], ALU.mult, [B_qn, B_tabs], [B_t1])
                    tt("dve", t2[lo:hi, :], pr[lo:hi, :], tabs[lo:hi, sini, pos0:pos0 + TG], ALU.mult, [B_pr, B_tabs], [B_t2])
                    rel(PSF, (pr, B_pr))
                    tt("pool", qn[lo:hi, :], t1[lo:hi, :], t2[lo:hi, :], ALU.add, [B_t1, B_t2], [B_qn])
                    rel(WF, (t1, B_t1), (t2, B_t2))

                def chainA(ci, t0=t0, B_xg=B_xg):
                    c0 = QA0 + ci * 128 if ci < 4 else KA0
                    gcol = 0 if ci < 4 else 1
                    (p, Bp), = yield from acq(PSF, 1)
                    projmm(p, Bp, 128, lambda kc: wqkv[:, kc, c0:c0 + 128], xrhs, 8, [B_xg, wq_buf(c0)])
                    (sq, B_sq), = yield from acq(HF, 1)
                    act(sq, p, AF.Square, [Bp], [B_sq])
                    yield
                    (pss, B_pss), = yield from acq(PSF, 1)
                    mm(pss, onesBD16, sq, True, True, [B_sq, B_mats16], [B_pss])
                    rel(HF, (sq, B_sq))
                    (rs, B_rs), = yield from acq(WF, 1)
                    act(rs, pss, AF.Ln, [B_pss, B_cvec], [B_rs], scale=1.0 / 64, bias=eps_ap)
                    rel(PSF, (pss, B_pss))
                    act(rs, rs, AF.Exp, [B_rs], [B_rs], scale=-0.5)
                    yield
                    (qn, B_qn), = yield from acq(HF, 1)
                    stt(qn, p, gv[:, gcol:gcol + 1], rs, ALU.mult, ALU.mult, [Bp, B_gv, B_rs], [B_qn])
                    rel(PSF, (p, Bp))
                    rel(WF, (rs, B_rs))
                    yield
                    yield from rope_gen(qn, B_qn, (0, 128), PA16, 0, 1)
                    if ci < 4:
                        S.dma("sp", qaT[ci, :, t0:t0 + TG], qn, reads=[B_qn], writes=[sb("qaT")])
                    else:
                        S.dma("sp", kaT[:, :, t0:t0 + TG].rearrange("k r t -> (k r) t"), qn, reads=[B_qn], writes=[sb("kaT")])
                    rel(HF, (qn, B_qn))

                def chainB(ci, t0=t0, B_xg=B_xg):
                    c0 = QB0 + ci * 128 if ci < 4 else KB0
                    (p, Bp), = yield from acq(PSF, 1)
                    projmm(p, Bp, 128, lambda kc: wqkv[:, kc, c0:c0 + 128], xrhs, 8, [B_xg, wq_buf(c0)])
                    (qn, B_qn), = yield from acq(HF, 1)
                    act(qn, p, AF.Copy, [Bp], [B_qn])
                    rel(PSF, (p, Bp))
                    if ci < 4:
                        S.dma("sp", qbT[ci, :, t0:t0 + TG], qn, reads=[B_qn], writes=[sb("qbT")])
                    else:
                        S.dma("sp", kbT[:, :, t0:t0 + TG].rearrange("k r t -> (k r) t"), qn, reads=[B_qn], writes=[sb("kbT")])
                    rel(HF, (qn, B_qn))

                def chainV(t0=t0, B_xg=B_xg, xT=xT):
                    (pv0, B_pv0), (pv1, B_pv1) = yield from acq(PSF, 2)
                    for t4 in range(4):
                        pv, B_pv = (pv0, B_pv0) if t4 < 2 else (pv1, B_pv1)
                        for vi, v0 in enumerate((VA0, VB0)):
                            for kc in range(8):
                                o_ = pv[:, (t4 % 2) * 256 + vi * 128:(t4 % 2) * 256 + (vi + 1) * 128]
                                mm(o_, xT[:, kc, t4 * 128:(t4 + 1) * 128], wqkv[:, kc, v0:v0 + 128], kc == 0, kc == 7,
                                   [B_xg, wq_buf(v0)], [B_pv])
                    vs, B_vs = vst.next()
                    act(vs[:, 0:2, :], pv0.rearrange("p (a b) -> p a b", a=2), AF.Copy, [B_pv0], [B_vs])
                    act(vs[:, 2:4, :], pv1.rearrange("p (a b) -> p a b", a=2), AF.Copy, [B_pv1], [B_vs])
                    rel(PSF, (pv0, B_pv0), (pv1, B_pv1))
                    for vi, vdst in enumerate((vA, vB)):
                        for t4 in range(4):
                            tok = t0 + t4 * 128
                            S.dma("sp", vdst[:, tok:tok + 128, :].rearrange("k t d -> t k d"),
                                  vs[:, t4, vi * 128:(vi + 1) * 128].rearrange("p (k d) -> p k d", k=2),
                                  reads=[B_vs], writes=[sb("vA" if vi == 0 else "vB")])

                def headQ(h, t0=t0):
                    (p, Bp), = yield from acq(PSF, 1)
                    projmm(p, Bp, 96, lambda kc: wuq[:, kc, h * 96:(h + 1) * 96], lambda kc: cqn[:, kc, :], 3, [B_cqn, B_wu])
                    (qh, B_qh), = yield from acq(HF, 1)
                    act(qh[0:96, :], p[0:96, :], AF.Copy, [Bp], [B_qh])
                    rel(PSF, (p, Bp))
                    yield
                    yield from rope_gen(qh, B_qh, (64, 96), PC16, 2, 3)
                    S.dma("sp", qcT[h, :, t0:t0 + TG], qh[0:96, :], reads=[B_qh], writes=[sb("qcT")])
                    rel(HF, (qh, B_qh))

                def norm_gen(c0, nch, sqt, B_sqt, dst, B_dst, gcol0, B_xg=B_xg):
                    banks = yield from acq(PSF, nch)
                    for c in range(nch):
                        p, Bp = banks[c]
                        projmm(p, Bp, 128, lambda kc, c=c: wqkv[:, kc, c0 + c * 128:c0 + (c + 1) * 128], xrhs, 8, [B_xg, wq_buf(c0)])
                        act(sqt[:, c, :], p, AF.Square, [Bp], [B_sqt])
                    yield
                    (pss, B_pss), = yield from acq(PSF, 1)
                    for c in range(nch):
                        mm(pss, ones16, sqt[:, c, :], c == 0, c == nch - 1, [B_sqt, B_mats16], [B_pss])
                    (rs, B_rs), = yield from acq(WF, 1)
                    act(rs, pss, AF.Ln, [B_pss, B_cvec], [B_rs], scale=1.0 / (128 * nch), bias=eps_ap)
                    rel(PSF, (pss, B_pss))
                    act(rs, rs, AF.Exp, [B_rs], [B_rs], scale=-0.5)
                    yield
                    for c in range(nch):
                        stt(dst[:, c, :], banks[c][0], gv[:, gcol0 + c:gcol0 + c + 1], rs, ALU.mult, ALU.mult, [banks[c][1], B_gv, B_rs], [B_dst])
                    rel(PSF, *banks)
                    rel(WF, (rs, B_rs))

                def chainCq():
                    yield from norm_gen(CQ0, 3, sq3, B_sq3, cqn, B_cqn, 2)
                    for h in range(8):
                        queue.insert(0, headQ(7 - h))

                def headK(h, t0=t0):
                    (p, Bp), = yield from acq(PSF, 1)
                    projmm(p, Bp, 64, lambda kc: wukv[:, kc, h * 128:h * 128 + 64], lambda kc: ckvn[:, kc, :], 2, [B_ckvn, B_wu])
                    (kh, B_kh), = yield from acq(HF, 1)
                    act(kh[0:64, :], p[0:64, :], AF.Copy, [Bp], [B_kh])
                    rel(PSF, (p, Bp))
                    S.dma("sp", kcT[h, 0:64, t0:t0 + TG], kh[0:64, :], reads=[B_kh], writes=[sb("kcT")])
                    rel(HF, (kh, B_kh))

                def tileVC(t4, t0=t0):
                    (pv, B_pv), = yield from acq(PSF, 1)
                    for c in range(2):
                        rhs = wukv[:, c, :].rearrange("p (h d) -> p h d", h=8)[:, :, 64:128]
                        mm(pv, ckvn[:, c, t4 * 128:(t4 + 1) * 128], rhs, c == 0, c == 1, [B_ckvn, B_wu], [B_pv])
                    vc, B_vc = vcst.next()
                    act(vc, pv, AF.Copy, [B_pv], [B_vc])
                    rel(PSF, (pv, B_pv))
                    tok = t0 + t4 * 128
                    S.dma("sp", vC[:, tok:tok + 128, :].rearrange("h t d -> t h d"), vc.rearrange("p (h d) -> p h d", h=8),
                          reads=[B_vc], writes=[sb("vC")])

                def chainCkv():
                    yield from norm_gen(CKV0, 2, sq3k, B_sq3k, ckvn, B_ckvn, 5)
                    for t4 in range(4):
                        queue.insert(0, tileVC(3 - t4))
                    for h in range(8):
                        queue.insert(0, headK(7 - h))

                def chainKpe(t0=t0, B_xg=B_xg):
                    (p, Bp), = yield from acq(PSF, 1)
                    projmm(p, Bp, 96, lambda kc: wkrp[:, kc, :], xrhs, 8, [B_xg, B_wkrp])
                    (kp, B_kp), = yield from acq(HF, 1)
                    act(kp[0:96, :], p[0:96, :], AF.Copy, [Bp], [B_kp])
                    rel(PSF, (p, Bp))
                    yield
                    yield from rope_gen(kp, B_kp, (64, 96), PC16, 2, 3)
                    for h in range(8):
                        S.dma("sp", kcT[h, 64:96, t0:t0 + TG], kp[64:96, :], reads=[B_kp], writes=[sb("kcT")])
                    rel(HF, (kp, B_kp))

                queue.extend([chainCq(), chainA(0), chainCkv(), chainB(0), chainA(1), chainKpe(), chainB(1), chainA(2),
                              chainV(), chainB(2), chainA(3), chainB(3), chainA(4), chainB(4)])
                run_gens(queue, width=4)
            S.barrier()
            A.release(m1)
            if stop_after == "p1":
                S.emit(es)
                return nc

            R1 = 135 * 1024
            wgh = A.at(R1, BF16, [8, 3, 1024])
            wbh = A.at(R1 + 48 * 1024, BF16, [3, 4, 1024])
            B_wgh = [Buf("wgh%d" % i) for i in range(2)]
            B_wbh = [Buf("wbh%d" % i) for i in range(2)]
            for half in range(2):
                for m in range(3):
                    c0 = GA0 + m * 1024 + half * 512
                    S.dma("pool", wgh[:, :, m, half * 512:(half + 1) * 512], w_l[:, :, c0:c0 + 512], writes=[B_wgh[half]])
                    S.dma("pool", wbh[:, m, :, half * 512:(half + 1) * 512],
                          w_br[m][l].rearrange("(kc p) c -> p kc c", p=128)[:, :, half * 512:(half + 1) * 512], writes=[B_wbh[half]])
            m2 = A.mark()
            PSA = Ring(psum[0:2])
            PSS = Ring(psum[2:8])
            biasT = A.alloc(F32, [2, 3 * 4 * 128])
            B_biasT = Buf("biasT")
            build_biasT(biasT, B_biasT)
            esr32 = A.alloc(F32, [16])
            esrow = A.alloc(BF16, [1024])
            B_es = Buf("es")
            S.dma("sp", esr32[0:1, 0:8], bass.AP(b_sink.tensor, l * 8, [[0, 1], [1, 8]]), writes=[B_es])
            act(esr32[0:1, 8:16], esr32[0:1, 0:8], AF.Exp, [B_es], [B_es])
            for j in range(2):
                for hh in range(2):
                    for pl in range(2):
                        h = 4 * j + 2 * pl + hh
                        off = ((j * 2 + pl) * 2 + hh) * 128
                        src = esr32[0:1, 8 + h:9 + h]
                        srcb = bass.AP(src.tensor, src.offset, [list(src.ap[0]), [0, 128]])
                        S.add("dve", lambda e, off=off, srcb=srcb: e.tensor_copy(out=esrow[0:1, off:off + 128], in_=srcb),
                              [B_es], [B_es])
            KTr = Ring([(A.alloc(BF16, [SEQ]), Buf("KT%d" % i)) for i in range(2)])
            QTr = Ring([(A.alloc(BF16, [4, SEQ]), Buf("QT%d" % i)) for i in range(2)])
            vaug_items = []
            for i in range(2):
                va_ = A.alloc(BF16, [16, 128])
                Bv = Buf("vaug%d" % i)
                S.add("pool", lambda e, va_=va_: e.memset(va_[:, :, 64:128], 1.0), writes=[Bv])
                vaug_items.append((va_, Bv))
            VAr = Ring(vaug_items)
            pTr = Ring([(A.alloc(BF16, [512]), Buf("pT%d" % i)) for i in range(6)])
            tmpr = Ring([(A.alloc(F32, [512]), Buf("tmp%d" % i)) for i in range(3)])
            rsr = Ring([(A.alloc(F32, [512]), Buf("rs%d" % i)) for i in range(2)])
            stg = Ring([(A.alloc(BF16, [4, SEQ]), Buf("stg%d" % i)) for i in range(2)])
            KTAr = []
            for i in range(2):
                kta = A.alloc(BF16, [2, SEQ])
                Bk = Buf("KTA%d" % i)
                S.add("pool", lambda e, kta=kta: e.memset(kta[64:128, 0, :], 0.0), writes=[Bk])
                S.add("pool", lambda e, kta=kta: e.memset(kta[0:64, 1, :], 0.0), writes=[Bk])
                KTAr.append((kta, Bk))
            KTAr = Ring(KTAr)
            LOOK = 3
            pending = []

            def emit_pv(st):
                mm(st["po"], st["vlhs"], st["pT"], st["first"], st["last"] and st["fin"] is None,
                   [st["B_pT"], st["B_v"]], [st["B_po"]])
                if st["last"]:
                    if st["sink"] is not None:
                        mm(st["po"], vsink16[0:1, :], st["sink"], False, True, [B_mats16, B_es], [st["B_po"]])
                    st["finalize"]()

            def run_step(st):
                pst, B_pst = PSS.next()
                n = len(st["smm"])
                for (cols, lhsT, rhs) in st["smm"]:
                    mm(pst[:, cols[0]:cols[1]], lhsT, rhs, True, True, st["reads_s"], [B_pst])
                pT, B_pT = pTr.next()
                if st["bias"] is None:
                    act(pT, pst, AF.Exp, [B_pst], [B_pT], scale=st["scale"])
                else:
                    tmp, B_tmp = tmpr.next()
                    stt(tmp, pst, st["scale"], st["bias"], ALU.mult, ALU.add, [B_pst, B_biasT], [B_tmp])
                    act(pT, tmp, AF.Exp, [B_tmp], [B_pT])
                st["pT"] = pT
                st["B_pT"] = B_pT
                pending.append(st)
                if len(pending) > LOOK:
                    emit_pv(pending.pop(0))

            def flush():
                while pending:
                    emit_pv(pending.pop(0))

            def make_finalize(po, B_po, dst, B_dst, shape3=None):
                def fin():
                    rs, B_rs = rsr.next()
                    if shape3 is not None:
                        act(rs[64:128, :], po[64:128, :], AF.Ln, [B_po], [B_rs])
                        act(rs[64:128, :], rs[64:128, :], AF.Exp, [B_rs], [B_rs], scale=-1.0)
                    else:
                        S.add("dve", lambda e: e.reciprocal(out=rs[64:128, :], in_=po[64:128, :]), [B_po], [B_rs])
                    a_ = po[0:64, :]
                    b_ = rs[64:128, :]
                    if shape3 is not None:
                        a_ = a_.rearrange("p (a b) -> p a b", a=shape3)
                        b_ = b_.rearrange("p (a b) -> p a b", a=shape3)
                    tt("dve", dst, a_, b_, ALU.mult, [B_po, B_rs], [B_dst])
                return fin

            import os
            dbg = os.environ.get("DBG_P2", "ABC")
            for s_ in range(NSEQ if "1" not in dbg else 1):
                tk0 = s_ * SEQ
                for mi, (qsrc, ksrc, vsrc, qn_, kn_, vn_) in enumerate(((qaT, kaT, vA, "qaT", "kaT", "vA"), (qbT, kbT, vB, "qbT", "kbT", "vB"))):
                    if "AB"[mi] not in dbg:
                        continue
                    for j in range(2):
                        QT, B_QT = QTr.next()
                        VA, B_VA = VAr.next()
                        if mi == 0:
                            KT, B_KT = KTAr.next()
                            for hf in range(2):
                                S.dma("sp", KT[hf * 64:(hf + 1) * 64, hf, :], ksrc[j, :, tk0:tk0 + SEQ], reads=[sb(kn_)], writes=[B_KT])
                        else:
                            KT, B_KT = KTr.next()
                            S.dma("sp", KT[0:64, :], ksrc[j, :, tk0:tk0 + SEQ], reads=[sb(kn_)], writes=[B_KT])
                        if mi == 0:
                            S.dma("sp", QT[:, 0:2, :], qsrc[2 * j:2 * j + 2, :, tk0:tk0 + SEQ].rearrange("a p t -> p a t"), reads=[sb(qn_)], writes=[B_QT])
                        else:
                            S.dma("sp", QT[0:64, :, :], qsrc[2 * j:2 * j + 2, :, tk0:tk0 + SEQ].rearrange("a (h r) t -> r (a h) t", h=2),
                                  reads=[sb(qn_)], writes=[B_QT])
                        S.dma("sp", VA[:, :, 0:64], vsrc[j, tk0:tk0 + SEQ, :].rearrange("(c p) d -> p c d", p=128), reads=[sb(vn_)], writes=[B_VA])
                        sg, B_sg = stg.next()
                        if mi == 0:
                            for qg in range(4):
                                for pl in range(2):
                                    for hh in range(2):
                                        po, B_po = PSA.next()
                                        fin = make_finalize(po, B_po, sg[0:64, pl * 2 + hh, qg * 512:(qg + 1) * 512], B_sg)
                                        for c in range(16):
                                            run_step(dict(
                                                smm=[((0, 512), KT[:, hh, c * 128:(c + 1) * 128],
                                                      QT[:, pl, qg * 512:(qg + 1) * 512])],
                                                reads_s=[B_KT, B_QT], bias=None, scale=0.125,
                                                po=po, B_po=B_po, vlhs=VA[:, c, :], B_v=B_VA, first=(c == 0), last=(c == 15),
                                                sink=None, fin=None, finalize=fin))
                            flush()
                            for pl in range(2):
                                for hh in range(2):
                                    S.dma("pool", oT[0, 2 * j + pl, hh * 64:(hh + 1) * 64, tk0:tk0 + SEQ], sg[0:64, pl * 2 + hh, :],
                                          reads=[B_sg], writes=[sb("oT")])
                        else:
                            for i in range(16 if 'T' not in dbg else 3):
                                po, B_po = PSA.next()
                                fin = make_finalize(po, B_po, sg[0:64, :, i * 128:(i + 1) * 128], B_sg, shape3=4)
                                cos_ = [co for co in (-1, 0, 1) if 0 <= i + co < 16]
                                for ci, co in enumerate(cos_):
                                    kb = i + co
                                    smm = [((0, 512), KT[0:64, kb * 128:(kb + 1) * 128], QT[0:64, :, i * 128:(i + 1) * 128])]
                                    run_step(dict(
                                        smm=smm, reads_s=[B_KT, B_QT], bias=biasT[:, j, (co + 1) * 512:(co + 2) * 512], scale=0.125,
                                        po=po, B_po=B_po, vlhs=VA[:, kb, :], B_v=B_VA, first=(ci == 0), last=(ci == len(cos_) - 1),
                                        sink=(esrow[0:1, j * 512:(j + 1) * 512] if "S" not in dbg else None), fin=(True if "S" not in dbg else None), finalize=fin))
                            flush()
                            for pl in range(2):
                                for hh in range(2):
                                    S.dma("pool", oT[1, 2 * j + pl, hh * 64:(hh + 1) * 64, tk0:tk0 + SEQ], sg[0:64, pl * 2 + hh, :],
                                          reads=[B_sg], writes=[sb("oT")])
                for h in range(8 if "C" in dbg else 0):
                    KT, B_KT = KTr.next()
                    QT, B_QT = QTr.next()
                    VA, B_VA = VAr.next()
                    S.dma("sp", KT[0:96, :], kcT[h, :, tk0:tk0 + SEQ], reads=[sb("kcT")], writes=[B_KT])
                    S.dma("sp", QT[0:96, 0, :], qcT[h, :, tk0:tk0 + SEQ], reads=[sb("qcT")], writes=[B_QT])
                    S.dma("sp", VA[:, :, 0:64], vC[h, tk0:tk0 + SEQ, :].rearrange("(c p) d -> p c d", p=128), reads=[sb("vC")], writes=[B_VA])
                    sg, B_sg = stg.next()
                    for qg in range(4):
                        po, B_po = PSA.next()
                        fin = make_finalize(po, B_po, sg[0:64, 0, qg * 512:(qg + 1) * 512], B_sg)
                        for c in range(16):
                            run_step(dict(
                                smm=[((0, 512), KT[0:96, c * 128:(c + 1) * 128], QT[0:96, 0, qg * 512:(qg + 1) * 512])],
                                reads_s=[B_KT, B_QT], bias=None, scale=96.0 ** -0.5,
                                po=po, B_po=B_po, vlhs=VA[:, c, :], B_v=B_VA, first=(c == 0), last=(c == 15),
                                sink=None, fin=None, finalize=fin))
                    flush()
                    S.dma("pool", oT[2, h // 2, (h % 2) * 64:(h % 2 + 1) * 64, tk0:tk0 + SEQ], sg[0:64, 0, :], reads=[B_sg], writes=[sb("oT")])
            assert A.peak <= 135 * 1024 or True
            S.barrier()
            A.release(m2)
            if stop_after == "p2":
                S.emit(es)
                return nc

            m3 = A.mark()
            R2 = 100 * 1024
            wo = A.at(R2, BF16, [8, D])
            B_wo = Buf("wo")
            S.dma("pool", wo, w_o[l].rearrange("(kc p) c -> p kc c", p=128), writes=[B_wo])
            gbc1 = A.at(R2 + 16 * 1024, F32, [D])
            bbc1 = A.at(R2 + 20 * 1024, F32, [D])
            B_gb1 = Buf("gb1")
            S.dma("sp", gbc1, bcast_rows(ln1_g.tensor, l * D, D), writes=[B_gb1])
            S.dma("sp", bbc1, bcast_rows(ln1_b.tensor, l * D, D), writes=[B_gb1])
            xgr = Ring([(A.alloc(BF16, [8, TG]), Buf("xg%d" % i)) for i in range(2)])
            oinr = Ring([(A.alloc(BF16, [3, 4, TG]), Buf("oin%d" % i)) for i in range(2)])
            thr = Ring([(A.alloc(F32, [512]), Buf("th%d" % i)) for i in range(4)])
            amr = Ring([(A.alloc(F32, [512]), Buf("am%d" % i)) for i in range(6)])
            mstr = Ring([(A.alloc(BF16, [8, TG]), Buf("mst%d" % i)) for i in range(2)])
            for g in range(NG):
                t0 = g * TG
                xg, B_xg = xgr.next()
                S.dma("sp", xg, xTd[:, :, t0:t0 + TG].rearrange("k p t -> p k t"), reads=[sb("xTd%d" % (g // 4))], writes=[B_xg])
                oin, B_oin = oinr.next()
                for m in range(3):
                    S.dma("sp", oin[:, m, :, :], oT[m, :, :, t0:t0 + TG].rearrange("k p t -> p k t"), reads=[sb("oT")], writes=[B_oin])
                mst, B_mst = mstr.next()
                for dc in range(8):
                    hf = dc // 4
                    ams = []
                    for m in range(3):
                        py, B_py = PS.next()
                        for kc in range(4):
                            mm(py, wbh[:, m, kc, dc * 128:(dc + 1) * 128], oin[:, m, kc, :], kc == 0, kc == 3, [B_wbh[hf], B_oin], [B_py])
                        pg, B_pg = PS.next()
                        for kc in range(8):
                            mm(pg, wgh[:, kc, m, dc * 128:(dc + 1) * 128], xg[:, kc, :], kc == 0, kc == 7, [B_wgh[hf], B_xg], [B_pg])
                        th, B_th = thr.next()
                        act(th, pg, AF.Tanh, [B_pg], [B_th], scale=0.5)
                        am, B_am = amr.next()
                        stt(am, th, 1.0, py, ALU.add, ALU.mult, [B_th, B_py], [B_am])
                        ams.append((am, B_am))
                    tt("dve", ams[0][0], ams[0][0], ams[1][0], ALU.add, [ams[0][1], ams[1][1]], [ams[0][1]])
                    tt("dve", mst[:, dc, :], ams[0][0], ams[2][0], ALU.add, [ams[0][1], ams[2][1]], [B_mst])
                S.dma("pool", mTd[:, :, t0:t0 + TG].rearrange("k p t -> p k t"), mst, reads=[B_mst], writes=[sb("mTd")])
            S.barrier()
            A.release(m3)
            if stop_after == "p3a":
                S.emit(es)
                return nc

            m3 = A.mark()
            gbc, bbc, B_gb = gbc1, bbc1, B_gb1
            rt2 = A.alloc(F32, [9, 64])
            B_rt2 = Buf("rt2")
            lnw = make_ln_work(2)
            mgr = Ring([(A.alloc(BF16, [8, TG]), Buf("mg%d" % i)) for i in range(2)])
            xrr = Ring([(A.alloc(F32, [D]), Buf("xr%d" % i)) for i in range(2)])
            yr = Ring([(A.alloc(F32, [D]), Buf("y%d" % i)) for i in range(2)])
            xstr = Ring([(A.alloc(BF16, [8, TG]), Buf("xst%d" % i)) for i in range(2)])
            for g in range(NG):
                t0 = g * TG
                mg, B_mg = mgr.next()
                S.dma("sp", mg, mTd[:, :, t0:t0 + TG].rearrange("k p t -> p k t"), reads=[sb("mTd")], writes=[B_mg])
                xst, B_xst = xstr.next()
                for t4 in range(4):
                    t = g * 4 + t4
                    xr, B_xr = xrr.next()
                    S.dma("sp", xr, xres[t * 128:(t + 1) * 128, :], reads=[B_xres[t]], writes=[B_xr])
                    y, B_y = yr.next()
                    for hf in range(2):
                        pw, B_pw = PS.next()
                        for kc in range(8):
                            mm(pw, mg[:, kc, t4 * 128:(t4 + 1) * 128], wo[:, kc, hf * 512:(hf + 1) * 512], kc == 0, kc == 7, [B_mg, B_wo], [B_pw])
                        stt(y[:, hf * 512:(hf + 1) * 512], xr[:, hf * 512:(hf + 1) * 512], 2.0 * ALPHA, pw, ALU.mult, ALU.add, [B_pw, B_xr], [B_y])
                    layer_norm_tile(y, B_y, gbc, bbc, B_gb, t, lnw, xres, xst, B_xst, t4, eps=4.0 * EPS)
                S.dma("pool", xTd[:, :, t0:t0 + TG].rearrange("k p t -> p k t"), xst, reads=[B_xst], writes=[sb("xTd%d" % (g // 4))])
                router_group(xst, B_xst, g, rt2, B_rt2)
            S.barrier()
            A.release(m3)
            if stop_after == "p3b":
                S.emit(es)
                return nc

            m4 = A.mark()
            last_layer = (l == L - 1)
            gbc, bbc, B_gb = load_gb(ln2_g.tensor, l * D, ln2_b.tensor, l * D)
            lnw = make_ln_work(2)
            x1T = A.alloc(BF16, [8, SEQ])
            B_x1T = Buf("x1T")
            acc = A.alloc(F32, [16, D])
            B_acc = [Buf("acc%d" % i) for i in range(16)]
            wexp = Ring([(A.alloc(BF16, [8, FF]), A.alloc(BF16, [8, FF]), A.alloc(BF16, [4, D]), Buf("wexp%d" % i)) for i in range(2)])
            hidr = Ring([(A.alloc(BF16, [4, TG]), Buf("hid%d" % i)) for i in range(2)])
            thr = Ring([(A.alloc(F32, [512]), Buf("th%d" % i)) for i in range(2)])
            xrr = Ring([(A.alloc(F32, [D]), Buf("xr%d" % i)) for i in range(2)])
            xstr = Ring([(A.alloc(BF16, [8, TG]), Buf("xst%d" % i)) for i in range(1)])
            for blk in range(NSEQ):
                tk0 = blk * SEQ
                for q4 in range(4):
                    S.dma("sp", x1T[:, :, q4 * 512:(q4 + 1) * 512], xTd[:, :, tk0 + q4 * 512:tk0 + (q4 + 1) * 512].rearrange("k p t -> p k t"),
                          reads=[sb("xTd%d" % blk)], writes=[B_x1T])
                for ex in range(NE):
                    wge, wue, wde, B_we = wexp.next()
                    S.dma("pool", wge, w_gate[l, ex].rearrange("(kc p) f -> p kc f", p=128), writes=[B_we])
                    S.dma("pool", wue, w_up[l, ex].rearrange("(kc p) f -> p kc f", p=128), writes=[B_we])
                    S.dma("pool", wde, w_down[l, ex].rearrange("(kc p) c -> p kc c", p=128), writes=[B_we])
                    for tg in range(4):
                        hid, B_hid = hidr.next()
                        for fc in range(4):
                            pg, B_pg = PS.next()
                            pu, B_pu = PS.next()
                            for kc in range(8):
                                mm(pg, wge[:, kc, fc * 128:(fc + 1) * 128], x1T[:, kc, tg * 512:(tg + 1) * 512], kc == 0, kc == 7, [B_we, B_x1T], [B_pg])
                            for kc in range(8):
                                mm(pu, wue[:, kc, fc * 128:(fc + 1) * 128], x1T[:, kc, tg * 512:(tg + 1) * 512], kc == 0, kc == 7, [B_we, B_x1T], [B_pu])
                            th, B_th = thr.next()
                            act(th, pg, AF.Tanh, [B_pg], [B_th], scale=0.5)
                            stt(th, th, 1.0, pg, ALU.add, ALU.mult, [B_th, B_pg], [B_th])
                            tt("dve", hid[:, fc, :], th, pu, ALU.mult, [B_th, B_pu], [B_hid])
                        for t4 in range(4):
                            t = tg * 4 + t4
                            for hf in range(2):
                                pd, B_pd = PS.next()
                                for fc in range(4):
                                    mm(pd, hid[:, fc, t4 * 128:(t4 + 1) * 128], wde[:, fc, hf * 512:(hf + 1) * 512], fc == 0, fc == 3, [B_hid, B_we], [B_pd])
                                a_ = acc[:, t, hf * 512:(hf + 1) * 512]
                                if ex == 0:
                                    ts("dve", a_, pd, comb_all[:, blk * 16 + t, ex:ex + 1], None, ALU.mult, ALU.bypass, [B_pd, B_comb], [B_acc[t]])
                                else:
                                    stt(a_, pd, comb_all[:, blk * 16 + t, ex:ex + 1], a_, ALU.mult, ALU.add, [B_pd, B_comb, B_acc[t]], [B_acc[t]])
                for tg in range(4):
                    xst, B_xst = xstr.next()
                    for t4 in range(4):
                        t = tg * 4 + t4
                        gt = blk * 16 + t
                        xr, B_xr = xrr.next()
                        S.dma("sp", xr, xres[gt * 128:(gt + 1) * 128, :], reads=[B_xres[gt]], writes=[B_xr])
                        stt(acc[:, t, :], xr, ALPHA, acc[:, t, :], ALU.mult, ALU.add, [B_xr, B_acc[t]], [B_acc[t]])
                        if last_layer:
                            layer_norm_tile(acc[:, t, :], B_acc[t], gbc, bbc, B_gb, gt, lnw, out)
                        else:
                            layer_norm_tile(acc[:, t, :], B_acc[t], gbc, bbc, B_gb, gt, lnw, xres, xst, B_xst, t4)
                    if not last_layer:
                        g = blk * 4 + tg
                        S.dma("pool", xTd[:, :, g * TG:(g + 1) * TG].rearrange("k p t -> p k t"), xst, reads=[B_xst], writes=[sb("xTd%d" % blk)])
            S.barrier()
            A.release(m4)
            if stop_after == "l%d" % l:
                S.emit(es)
                return nc
        S.emit(es)
    return nc
    return nc


_CONSTS = None


def kernel(**inputs):
    global _CONSTS
    if _CONSTS is None:
        _CONSTS = host_constants()
    x = np.ascontiguousarray(np.asarray(inputs["x"], dtype=np.float32))
    n_cores = 8
    base = {k: np.ascontiguousarray(np.asarray(v, dtype=np.float32)) for k, v in inputs.items() if k != "x"}
    base.update(_CONSTS)
    in_maps = []
    for c in range(n_cores):
        m = dict(base)
        m["x"] = np.ascontiguousarray(x[c * NSEQ:(c + 1) * NSEQ].reshape(NT, D))
        in_maps.append(m)
    nc = build_program()
    res = run_bass_kernel_spmd(nc, in_maps, core_ids=list(range(n_cores)))
    outs = [np.asarray(r["out"], dtype=np.float32).reshape(NSEQ, SEQ, D) for r in res.results]
    return np.concatenate(outs, axis=0)
```
